# Optimizing a Trainium2 kernel written in Bass

```python
import jax
import jax.numpy as jnp
from jax import lax
import numpy as np


D_MODEL = 1024
BATCH = 4
SEQ = 4096
DEPTH = 1

D_MIX = D_MODEL
RWKV_WIDTH = D_MIX // 2
RWKV_HEAD_DIM = 64
RWKV_HEADS = RWKV_WIDTH // RWKV_HEAD_DIM
DECAY_LORA = 64
ICLR_LORA = 64
GATE_LORA = 128
GLA_V_WIDTH = D_MIX - RWKV_WIDTH
GLA_HEADS = 4
GLA_DV = GLA_V_WIDTH // GLA_HEADS
GLA_DK = GLA_DV // 2
GLA_K_WIDTH = GLA_HEADS * GLA_DK
GLA_GATE_LORA = 16
GLA_TAU = 16.0
GLA_CHUNK = 16
N_GROUPS = 4
EXPERTS_PER_GROUP = 8
N_EXPERTS = N_GROUPS * EXPERTS_PER_GROUP
TOP_K = 2
D_EXPERT = 512
MOE_BLOCK = 128
RMS_EPS = 1e-6
RWKV_GN_EPS = 64e-5

RWKV_SPLITS = (RWKV_WIDTH, RWKV_WIDTH, RWKV_WIDTH, DECAY_LORA, DECAY_LORA, ICLR_LORA, ICLR_LORA, GATE_LORA)
GLA_SPLITS = (GLA_K_WIDTH, GLA_K_WIDTH, GLA_V_WIDTH, GLA_GATE_LORA, GLA_GATE_LORA, GLA_V_WIDTH)
RWKV_COLS = sum(RWKV_SPLITS)
D_IN_PROJ = RWKV_COLS + sum(GLA_SPLITS)

kernel_name = 'bidir_hybrid_rwkv7_gla_hmoe'


def _split(p, sizes):
    return jnp.split(p, np.cumsum(sizes)[:-1].tolist(), axis=-1)


def rmsnorm(x, w):
    xf = x.astype(jnp.float32)
    y = xf * lax.rsqrt(jnp.mean(xf * xf, axis=-1, keepdims=True) + RMS_EPS)
    return (y * w.astype(jnp.float32)).astype(x.dtype)


def centred_shift(p):
    prev = jnp.pad(p[:, :-1], ((0, 0), (1, 0), (0, 0)))
    nxt = jnp.pad(p[:, 1:], ((0, 0), (0, 1), (0, 0)))
    return 0.5 * (prev + nxt)


def rwkv7_bidir_scan(r, w, k, v, kk, b):
    def to_dir(z):
        z = jnp.stack([z[0], jnp.flip(z[1], axis=1)])
        return jnp.moveaxis(z, 2, 0)

    def both(z):
        return to_dir(jnp.stack([z, z]))

    xs = (both(r), to_dir(w), to_dir(k), both(v), both(kk), to_dir(b))
    bsz, _, h, n = r.shape
    s0 = jnp.zeros((2, bsz, h, n, n), jnp.float32)

    def step(s, inp):
        r_t, w_t, k_t, v_t, kk_t, b_t = inp
        sa = jnp.einsum('dbhvk,dbhk->dbhv', s, -kk_t)
        s = s * w_t[..., None, :] + sa[..., None] * b_t[..., None, :] + v_t[..., None] * k_t[..., None, :]
        return s, jnp.einsum('dbhvk,dbhk->dbhv', s, r_t)

    _, ys = lax.scan(step, s0, xs)
    ys = jnp.moveaxis(ys, 0, 2)
    return ys[0] + jnp.flip(ys[1], axis=1)


def rwkv7_mixer(p, mu, w0_f, w2_f, w0_b, w2_b, a0_f, a2_f, a0_b, a2_b, g2, k_k, k_a, r_k, ln_w, ln_b):
    p = p.astype(jnp.float32)
    p = p + mu * (centred_shift(p) - p)
    r, k, v, wl_f, wl_b, al_f, al_b, gl = _split(p, RWKV_SPLITS)

    def decay(w0, wl, w2):
        logw = -jax.nn.softplus(-(w0 + jnp.tanh(wl) @ w2)) - 0.5
        return jnp.exp(-jnp.exp(logw))

    def heads(z):
        return z.reshape(z.shape[:-1] + (RWKV_HEADS, RWKV_HEAD_DIM))

    w = jnp.stack([decay(w0_f, wl_f, w2_f), decay(w0_b, wl_b, w2_b)])
    a = jax.nn.sigmoid(jnp.stack([a0_f + al_f @ a2_f, a0_b + al_b @ a2_b]))
    g = jax.nn.sigmoid(gl) @ g2
    kk = heads(k * k_k)
    kk = kk / jnp.maximum(jnp.sqrt(jnp.sum(kk * kk, axis=-1, keepdims=True)), 1e-12)
    k_dir = k * (1.0 + (a - 1.0) * k_a)
    b = kk * heads(a)
    rh, vh = heads(r), heads(v)
    y = rwkv7_bidir_scan(rh, heads(w), heads(k_dir), vh, kk, b)
    mean = jnp.mean(y, axis=-1, keepdims=True)
    var = jnp.var(y, axis=-1, keepdims=True)
    y = (y - mean) * lax.rsqrt(var + RWKV_GN_EPS)
    y = y.reshape(y.shape[:-2] + (RWKV_WIDTH,)) * ln_w + ln_b
    bonus = jnp.sum(rh * heads(k_dir[0] + k_dir[1]) * r_k, axis=-1, keepdims=True) * vh
    return (y + bonus.reshape(y.shape)) * g


def gla_chunked(q, k, v, g):
    bsz, h, t, dk = q.shape
    dv = v.shape[-1]
    c = GLA_CHUNK
    n = t // c
    q, k, g = (z.reshape(bsz, h, n, c, dk) for z in (q, k, g))
    v = v.reshape(bsz, h, n, c, dv)
    bc = jnp.cumsum(g, axis=3)
    causal = jnp.tril(jnp.ones((c, c), dtype=bool))
    diff = bc[..., :, None, :] - bc[..., None, :, :]
    dec = jnp.exp(jnp.where(causal[:, :, None], diff, -jnp.inf))
    att = jnp.einsum('bhnid,bhnjd,bhnijd->bhnij', q, k, dec)
    o = jnp.einsum('bhnij,bhnjv->bhniv', att, v)
    b_last = bc[..., -1, :]
    u = jnp.einsum('bhncd,bhncv->nbhdv', k * jnp.exp(b_last[..., None, :] - bc), v)

    def step(s, inp):
        u_n, d_n = inp
        return s * d_n[..., None] + u_n, s

    s0 = jnp.zeros((bsz, h, dk, dv), jnp.float32)
    _, s_in = lax.scan(step, s0, (u, jnp.moveaxis(jnp.exp(b_last), 2, 0)))
    o = o + jnp.einsum('bhncd,nbhdv->bhncv', q * jnp.exp(bc), s_in)
    return o.reshape(bsz, h, t, dv)


def gla_mixer(p, gw2_f, gb_f, gw2_b, gb_b, norm_w):
    p = p.astype(jnp.float32)
    bsz, t, _ = p.shape
    q, k, v, gl_f, gl_b, og = _split(p, GLA_SPLITS)

    def heads(z, d):
        return jnp.moveaxis(z.reshape(z.shape[:-1] + (GLA_HEADS, d)), 2, 1)

    q = heads(q, GLA_DK) * (GLA_DK ** -0.5)
    k = heads(k, GLA_DK)
    v = heads(v, GLA_DV)
    g_f = heads(jax.nn.log_sigmoid(gl_f @ gw2_f + gb_f) / GLA_TAU, GLA_DK)
    g_b = heads(jax.nn.log_sigmoid(gl_b @ gw2_b + gb_b) / GLA_TAU, GLA_DK)

    def flip(z):
        return jnp.flip(z, axis=2)

    o = gla_chunked(q, k, v, g_f) + flip(gla_chunked(flip(q), flip(k), flip(v), flip(g_b)))
    o = o * lax.rsqrt(jnp.mean(o * o, axis=-1, keepdims=True) + RMS_EPS)
    o = jnp.moveaxis(o, 1, 2).reshape(bsz, t, GLA_V_WIDTH)
    return o * norm_w * jax.nn.silu(og)


def hier_moe(h, w_coarse, b_coarse, w_fine, b_fine, w_gate, w_up, w_down):
    bsz, t, d = h.shape
    n_tok = bsz * t
    hf = h.reshape(n_tok, d)
    tok_idx = jnp.arange(n_tok)
    coarse = (hf @ w_coarse + b_coarse).astype(jnp.float32)
    pc = jax.nn.softmax(coarse, axis=-1)
    gsel = jnp.argmax(coarse, axis=-1)
    pg = pc[tok_idx, gsel]
    fine = (hf @ w_fine + b_fine).astype(jnp.float32).reshape(n_tok, N_GROUPS, EXPERTS_PER_GROUP)
    pf = jax.nn.softmax(fine[tok_idx, gsel], axis=-1)
    top_p, top_e = lax.top_k(pf, TOP_K)
    comb = pg[:, None] * top_p / jnp.sum(top_p, axis=-1, keepdims=True)
    eid = (gsel[:, None] * EXPERTS_PER_GROUP + top_e).reshape(-1).astype(jnp.int32)
    n_asg = n_tok * TOP_K
    tok = jnp.repeat(tok_idx, TOP_K).astype(jnp.int32)
    wts = comb.reshape(-1)
    order = jnp.argsort(eid, stable=True)
    se, st, sw = eid[order], tok[order], wts[order]
    counts = jnp.bincount(eid, length=N_EXPERTS).astype(jnp.int32)
    starts = jnp.cumsum(counts) - counts
    padded = ((counts + MOE_BLOCK - 1) // MOE_BLOCK) * MOE_BLOCK
    pends = jnp.cumsum(padded)
    pstarts = pends - padded
    dest = pstarts[se] + jnp.arange(n_asg, dtype=jnp.int32) - starts[se]
    n_blocks = -(-n_asg // MOE_BLOCK) + N_EXPERTS
    n_rows = n_blocks * MOE_BLOCK
    row_tok = jnp.zeros((n_rows,), jnp.int32).at[dest].set(st)
    row_w = jnp.zeros((n_rows,), jnp.float32).at[dest].set(sw)
    block_e = jnp.minimum(jnp.searchsorted(pends, jnp.arange(n_blocks, dtype=jnp.int32) * MOE_BLOCK, side='right'), N_EXPERTS - 1)
    xs = hf[row_tok].reshape(n_blocks, MOE_BLOCK, d)

    def expert_block(args):
        xb, e = args
        return (jax.nn.silu(xb @ w_gate[e]) * (xb @ w_up[e])) @ w_down[e]

    ys = lax.map(expert_block, (xs, block_e)).reshape(n_rows, d)
    out = jax.ops.segment_sum(ys * row_w[:, None].astype(ys.dtype), row_tok, num_segments=n_tok)
    return out.reshape(bsz, t, d)


def setup_inputs(seed: int = 0) -> dict:
    key = jax.random.key(seed)
    ks = iter(jax.random.split(key, 40))

    def nrm(shape, scale):
        return jax.random.normal(next(ks), shape, jnp.float32) * scale

    def unif(shape, lo, hi):
        return jax.random.uniform(next(ks), shape, jnp.float32, lo, hi)

    L, D = DEPTH, D_MODEL
    return {
        'x': nrm((BATCH, SEQ, D), 1.0),
        'ln1_w': 1.0 + nrm((L, D), 0.05),
        'w_in': nrm((L, D, D_IN_PROJ), D ** -0.5),
        'rw_mu': unif((L, RWKV_COLS), 0.0, 1.0),
        'rw_w0_f': unif((L, RWKV_WIDTH), -6.0, -1.0),
        'rw_w2_f': nrm((L, DECAY_LORA, RWKV_WIDTH), 0.1),
        'rw_w0_b': unif((L, RWKV_WIDTH), -6.0, -1.0),
        'rw_w2_b': nrm((L, DECAY_LORA, RWKV_WIDTH), 0.1),
        'rw_a0_f': nrm((L, RWKV_WIDTH), 0.1),
        'rw_a2_f': nrm((L, ICLR_LORA, RWKV_WIDTH), 0.5 * ICLR_LORA ** -0.5),
        'rw_a0_b': nrm((L, RWKV_WIDTH), 0.1),
        'rw_a2_b': nrm((L, ICLR_LORA, RWKV_WIDTH), 0.5 * ICLR_LORA ** -0.5),
        'rw_g2': nrm((L, GATE_LORA, RWKV_WIDTH), GATE_LORA ** -0.5),
        'rw_k_k': 0.85 + nrm((L, RWKV_WIDTH), 0.05),
        'rw_k_a': 1.0 + nrm((L, RWKV_WIDTH), 0.05),
        'rw_r_k': nrm((L, RWKV_HEADS, RWKV_HEAD_DIM), 0.1),
        'rw_ln_w': 1.0 + nrm((L, RWKV_WIDTH), 0.05),
        'rw_ln_b': nrm((L, RWKV_WIDTH), 0.01),
        'gla_gw2_f': nrm((L, GLA_GATE_LORA, GLA_K_WIDTH), GLA_GATE_LORA ** -0.5),
        'gla_gb_f': 2.0 + nrm((L, GLA_K_WIDTH), 0.5),
        'gla_gw2_b': nrm((L, GLA_GATE_LORA, GLA_K_WIDTH), GLA_GATE_LORA ** -0.5),
        'gla_gb_b': 2.0 + nrm((L, GLA_K_WIDTH), 0.5),
        'gla_norm_w': 1.0 + nrm((L, GLA_V_WIDTH), 0.05),
        'w_out': nrm((L, D_MIX, D), D_MIX ** -0.5),
        'ln2_w': 1.0 + nrm((L, D), 0.05),
        'moe_w_coarse': nrm((L, D, N_GROUPS), D ** -0.5),
        'moe_b_coarse': nrm((L, N_GROUPS), 0.01),
        'moe_w_fine': nrm((L, D, N_EXPERTS), D ** -0.5),
        'moe_b_fine': nrm((L, N_EXPERTS), 0.01),
        'moe_w_gate': nrm((L, N_EXPERTS, D, D_EXPERT), D ** -0.5),
        'moe_w_up': nrm((L, N_EXPERTS, D, D_EXPERT), D ** -0.5),
        'moe_w_down': nrm((L, N_EXPERTS, D_EXPERT, D), D_EXPERT ** -0.5),
        'ln_f_w': 1.0 + nrm((D,), 0.05),
    }


def reference(x, ln1_w, w_in, rw_mu, rw_w0_f, rw_w2_f, rw_w0_b, rw_w2_b, rw_a0_f, rw_a2_f, rw_a0_b, rw_a2_b, rw_g2, rw_k_k, rw_k_a, rw_r_k, rw_ln_w, rw_ln_b, gla_gw2_f, gla_gb_f, gla_gw2_b, gla_gb_b, gla_norm_w, w_out, ln2_w, moe_w_coarse, moe_b_coarse, moe_w_fine, moe_b_fine, moe_w_gate, moe_w_up, moe_w_down, ln_f_w):
    for l in range(DEPTH):
        h = rmsnorm(x, ln1_w[l])
        p = h @ w_in[l]
        y_rw = rwkv7_mixer(p[..., :RWKV_COLS], rw_mu[l], rw_w0_f[l], rw_w2_f[l], rw_w0_b[l], rw_w2_b[l],
                           rw_a0_f[l], rw_a2_f[l], rw_a0_b[l], rw_a2_b[l], rw_g2[l], rw_k_k[l], rw_k_a[l],
                           rw_r_k[l], rw_ln_w[l], rw_ln_b[l])
        y_gla = gla_mixer(p[..., RWKV_COLS:], gla_gw2_f[l], gla_gb_f[l], gla_gw2_b[l], gla_gb_b[l], gla_norm_w[l])
        mix = jnp.concatenate([y_rw, y_gla], axis=-1).astype(x.dtype)
        x = x + mix @ w_out[l]
        x = x + hier_moe(rmsnorm(x, ln2_w[l]), moe_w_coarse[l], moe_b_coarse[l], moe_w_fine[l], moe_b_fine[l],
                         moe_w_gate[l], moe_w_up[l], moe_w_down[l])
    return rmsnorm(x, ln_f_w)
```

```python
import contextlib
import numpy as np
import ml_dtypes
import concourse.bass as bass
import concourse.mybir as mybir
from concourse.bass_utils import run_bass_kernel_spmd

F32 = mybir.dt.float32
BF16 = mybir.dt.bfloat16
AF = mybir.ActivationFunctionType
ALU = mybir.AluOpType

SAME_ENGINE_SYNC = True
D = 1024
NCOL = 1984
NCV = 25
CS_RW = -0.6065306597126334
CS_GL = 1.0 / 16.0
NEXP = 32
DEXP = 512


class _Stop(Exception):
    pass


class Dep:
    __slots__ = ("w", "r", "excl")

    def __init__(self):
        self.w = None
        self.r = {}
        self.excl = False


class Tl:
    def __init__(self, t, excl=False):
        self.t = t
        self.d = Dep()
        self.d.excl = excl

    def __getitem__(self, idx):
        return self.t[idx]


class Sched:
    ENGS = ("pe", "dve", "act", "pool", "sp")
    NDMA = 6

    def __init__(self, nc, es):
        self.nc = nc
        self.es = es
        self.q = {e: [] for e in self.ENGS}
        self.sems = {}
        self.cnt = {}
        self.cur = {}
        self.epoch = 0
        for e in self.ENGS:
            k = e + "#0"
            self.sems[k] = es.enter_context(nc.semaphore("s_" + e + "_0"))
            self.cnt[k] = 0
            self.cur[e] = k
        self.dma_rr = {"sp": 0, "pool": 0}
        for qn in ("sp", "pool"):
            for i in range(self.NDMA):
                k = "d_%s%d" % (qn, i)
                self.sems[k] = es.enter_context(nc.semaphore(k))
                self.cnt[k] = 0
        self.known = {e: {} for e in self.ENGS}

    def _collect(self, eng, R, W):
        need = {}

        def add(tok):
            if tok is None:
                return
            k, v = tok
            if need.get(k, 0) < v:
                need[k] = v
        for d in R:
            add(d.w)
        for d in W:
            add(d.w)
            for k, v in d.r.items():
                add((k, v))
        out = []
        kn = self.known[eng]
        for k, v in need.items():
            if k == self.cur[eng] and (eng == "pe" or not SAME_ENGINE_SYNC):
                continue
            if kn.get(k, 0) >= v:
                continue
            kn[k] = v
            out.append((k, v))
        return out

    def op(self, eng, fn, R=(), W=()):
        R = [x.d if isinstance(x, Tl) else x for x in R]
        W = [x.d if isinstance(x, Tl) else x for x in W]
        if eng != "pe":
            W = W + [x for x in R if x.excl and x not in W]
            R = [x for x in R if not x.excl]
        waits = self._collect(eng, R, W)
        key = self.cur[eng]
        self.cnt[key] += 1
        tok = (key, self.cnt[key])
        self.q[eng].append((waits, fn, key, 1))
        for d in W:
            d.w = tok
            d.r = {}
        for d in R:
            if d.r.get(key, 0) < tok[1]:
                d.r[key] = tok[1]
        return tok

    def dma(self, qn, fn, R=(), W=()):
        R = [x.d if isinstance(x, Tl) else x for x in R]
        W = [x.d if isinstance(x, Tl) else x for x in W]
        i = self.dma_rr[qn]
        self.dma_rr[qn] = (i + 1) % self.NDMA
        k = "d_%s%d" % (qn, i)
        waits = self._collect(qn, R, W)
        if self.cnt[k] > 0 and self.known[qn].get(k, 0) < self.cnt[k]:
            waits.append((k, self.cnt[k]))
            self.known[qn][k] = self.cnt[k]
        self.cnt[k] += 16
        tok = (k, self.cnt[k])
        self.q[qn].append((waits, fn, k, 16))
        for d in W:
            d.w = tok
            d.r = {}
        for d in R:
            if d.r.get(k, 0) < tok[1]:
                d.r[k] = tok[1]
        return tok

    def barrier(self):
        final = {k: v for k, v in self.cnt.items() if v > 0}
        for e in self.ENGS:
            waits = []
            for k, v in final.items():
                if k == self.cur[e] and e == "pe":
                    continue
                if self.known[e].get(k, 0) < v:
                    self.known[e][k] = v
                    waits.append((k, v))
            if waits:
                self.q[e].append((waits, None, None, 0))
        self.epoch += 1
        for e in self.ENGS:
            if e == "sp":
                continue
            old = self.cur[e]
            self.known[e][old] = max(self.known[e].get(old, 0), self.cnt[old])
            k = "%s#%d" % (e, self.epoch)
            self.sems[k] = self.es.enter_context(self.nc.semaphore("s_%s_%d" % (e, self.epoch)))
            self.cnt[k] = 0
            self.cur[e] = k

    def emit(self):
        nc = self.nc
        sems = self.sems
        q = self.q
        with nc.Block() as block:
            def run(engh, lst):
                for waits, fn, k, inc in lst:
                    for (wk, wv) in waits:
                        engh.wait_ge(sems[wk], wv)
                    if fn is not None:
                        ins = fn(engh)
                        ins.then_inc(sems[k], inc)

            @block.tensor
            def _(e):
                run(e, q["pe"])

            @block.vector
            def _(e):
                run(e, q["dve"])

            @block.scalar
            def _(e):
                run(e, q["act"])

            @block.gpsimd
            def _(e):
                run(e, q["pool"])

            @block.sync
            def _(e):
                run(e, q["sp"])


class KB:
    def __init__(self, nc, S):
        self.nc = nc
        self.S = S

    def mm(self, out, lhsT, rhs, start, stop, R, W):
        self.S.op("pe", lambda e: e.matmul(out, lhsT=lhsT, rhs=rhs, start=start, stop=stop), R, W)

    def tr(self, out, in_, ident, R, W):
        self.S.op("pe", lambda e: e.transpose(out, in_, ident), R, W)

    def act(self, out, in_, func, R, W, scale=1.0, bias=None, accum=None):
        def f(e):
            kw = {}
            if bias is not None:
                kw["bias"] = bias
            if accum is not None:
                kw["accum_out"] = accum
            return e.activation(out=out, in_=in_, func=func, scale=scale, **kw)
        self.S.op("act", f, R, W)

    def tt(self, out, a, b, op, R, W, eng="dve"):
        self.S.op(eng, lambda e: e.tensor_tensor(out=out, in0=a, in1=b, op=op), R, W)

    def ts(self, out, a, s1, op0, R, W, s2=None, op1=None, eng="dve"):
        if op1 is None:
            self.S.op(eng, lambda e: e.tensor_scalar(out=out, in0=a, scalar1=s1, scalar2=None, op0=op0), R, W)
        else:
            self.S.op(eng, lambda e: e.tensor_scalar(out=out, in0=a, scalar1=s1, scalar2=s2, op0=op0, op1=op1), R, W)

    def stt(self, out, in0, scalar, in1, op0, op1, R, W):
        self.S.op("dve", lambda e: e.scalar_tensor_tensor(out=out, in0=in0, scalar=scalar, in1=in1, op0=op0, op1=op1), R, W)

    def cp(self, out, in_, R, W, eng="act"):
        if eng == "act":
            self.S.op("act", lambda e: e.copy(out=out, in_=in_), R, W)
        else:
            self.S.op(eng, lambda e: e.tensor_copy(out=out, in_=in_), R, W)

    def red(self, out, in_, op, R, W):
        self.S.op("dve", lambda e: e.tensor_reduce(out=out, in_=in_, axis=mybir.AxisListType.X, op=op), R, W)

    def recip(self, out, in_, R, W):
        self.S.op("dve", lambda e: e.reciprocal(out=out, in_=in_), R, W)

    def scan(self, out, d0, d1, R, W):
        self.S.op("dve", lambda e: e.tensor_tensor_scan(out=out, data0=d0, data1=d1, initial=0.0, op0=ALU.mult, op1=ALU.add), R, W)

    def memset(self, ap, val, W, eng="pool"):
        self.S.op(eng, lambda e: e.memset(ap, val), (), W)

    def dma(self, out, in_, R, W, q="sp"):
        self.S.dma(q, lambda e: e.dma_start(out=out, in_=in_), R, W)


def build_nc(T, debug=False, stop=99):
    NT = T // 128
    NBLK = T // 512
    TH = T // 2
    NT2 = TH // 128
    nc = bass.Bass("TRN2", target_bir_lowering=False)
    dt = nc.dram_tensor
    xfull = dt("xfull", [T, D], F32, kind="ExternalInput").ap()
    xhalf = dt("xhalf", [TH, D], F32, kind="ExternalInput").ap()
    win = dt("win", [D, NCOL], F32, kind="ExternalInput").ap()
    cvec = dt("cvec", [128, NCV], F32, kind="ExternalInput").ap()
    rowv1 = dt("rowv1", [128, 768 + D], F32, kind="ExternalInput").ap()
    rowv2 = dt("rowv2", [128, 2 * D + 36 + 2], F32, kind="ExternalInput").ap()
    lora = dt("lora", [128, 3 * 256 + 128], F32, kind="ExternalInput").ap()
    cmask = dt("cmask", [128, 2 * 512 + 2 * 256 + 128 + 512 + 128 + 2], F32, kind="ExternalInput").ap()
    wo = dt("wo", [D, D], F32, kind="ExternalInput").ap()
    wr = dt("wr", [D, 36], F32, kind="ExternalInput").ap()
    if debug != 1:
        wg = dt("wg", [NEXP, D, DEXP], F32, kind="ExternalInput").ap()
        wu = dt("wu", [NEXP, D, DEXP], F32, kind="ExternalInput").ap()
        wd = dt("wd", [NEXP, DEXP, D], F32, kind="ExternalInput").ap()
    out = dt("out", [TH, D], F32, kind="ExternalOutput").ap()
    hT_d = dt("hT_d", [D, T], BF16, kind="Internal")
    QS = min(T, 1024)
    NQ = T // QS
    cc_in = [dt("cc_in%d" % q_, [512, QS], BF16, kind="Internal") for q_ in range(NQ)]
    cc_out = [dt("cc_out%d" % q_, [1024, QS], BF16, kind="Internal") for q_ in range(NQ)]
    if debug:
        dbg_mix = dt("dbg_mix", [T, 512], F32, kind="ExternalOutput").ap()
    d_hT = Dep()
    d_ccin = Dep()
    d_ccout = Dep()

    with contextlib.ExitStack() as es:
        S = Sched(nc, es)
        K = KB(nc, S)

        def sbt(es_, name, shape, dtype):
            return Tl(es_.enter_context(nc.sbuf_tensor(name, shape, dtype)))

        def pst(es_, name, shape, dtype):
            return Tl(es_.enter_context(nc.psum_tensor(name, shape, dtype)), excl=True)

        maskF = sbt(es, "maskF", [128, 512], BF16)
        maskB = sbt(es, "maskB", [128, 512], BF16)
        maskLF = sbt(es, "maskLF", [128, 256], BF16)
        maskLB = sbt(es, "maskLB", [128, 256], BF16)
        identb = sbt(es, "identb", [128, 128], BF16)
        identf = sbt(es, "identf", [128, 128], F32)
        rmask = sbt(es, "rmask", [128, 512], F32)
        bones = sbt(es, "bones", [128, 128], BF16)
        hsel = sbt(es, "hsel", [128, 2], BF16)
        ecm = contextlib.ExitStack()
        cm = sbt(ecm, "cm", [128, 2 * 512 + 2 * 256 + 128 + 512 + 128 + 2], F32)
        K.dma(cm[:], cmask, [], [cm])
        o = 0
        K.cp(maskF[:], cm[:, o:o + 512], [cm], [maskF], eng="dve"); o += 512
        K.cp(maskB[:], cm[:, o:o + 512], [cm], [maskB], eng="dve"); o += 512
        K.cp(maskLF[:], cm[:, o:o + 256], [cm], [maskLF], eng="dve"); o += 256
        K.cp(maskLB[:], cm[:, o:o + 256], [cm], [maskLB], eng="dve"); o += 256
        K.cp(identb[:], cm[:, o:o + 128], [cm], [identb], eng="dve")
        K.cp(identf[:], cm[:, o:o + 128], [cm], [identf], eng="dve"); o += 128
        K.cp(rmask[:], cm[:, o:o + 512], [cm], [rmask], eng="dve"); o += 512
        K.cp(bones[:], cm[:, o:o + 128], [cm], [bones], eng="dve"); o += 128
        K.cp(hsel[:], cm[:, o:o + 2], [cm], [hsel], eng="dve"); o += 2
        S.barrier()
        ecm.close()

        PJ0 = pst(es, "PJ0", [128, 512], F32)
        PJ1 = pst(es, "PJ1", [128, 512], F32)
        G0 = pst(es, "G0", [128, 512], F32)
        G1 = pst(es, "G1", [128, 512], F32)
        IA = pst(es, "IA", [128, 512], F32)
        IB = pst(es, "IB", [128, 512], F32)
        WUY = pst(es, "WUY", [128, 512], F32)
        TR = pst(es, "TR", [128, 1024], BF16)

        with contextlib.ExitStack() as e0:
            rv1 = sbt(e0, "rv1", [128, 768 + D], F32)
            K.dma(rv1[:], rowv1, [], [rv1])
            xts = [sbt(e0, "xt%d" % i, [128, D], F32) for i in range(2)]
            junk = sbt(e0, "junk", [128, D], BF16)
            hb = sbt(e0, "hb", [128, D], BF16)
            hts = [sbt(e0, "hts%d" % i, [128, 8, 128], BF16) for i in range(2)]
            ss = sbt(e0, "ss", [128, 1], F32)
            sd = sbt(e0, "sd", [128, 1], F32)
            rstd = sbt(e0, "rstd", [128, 1], F32)
            epsb = sbt(e0, "epsb", [128, 1], F32)
            K.memset(epsb[:], 1e-6, [epsb])
            for i in range(NT):
                xt = xts[i % 2]
                K.dma(xt[:], xfull[i * 128:(i + 1) * 128, :], [], [xt])
                K.act(junk[:], xt[:], AF.Square, [xt], [junk, ss], accum=ss[:])
                K.act(sd[:], ss[:], AF.Sqrt, [ss, epsb], [sd], scale=1.0 / D, bias=epsb[:])
                K.recip(rstd[:], sd[:], [sd], [rstd])
                K.stt(hb[:], xt[:], rstd[:, 0:1], rv1[:, 768:768 + D], ALU.mult, ALU.mult, [xt, rstd, rv1], [hb])
                for k in range(8):
                    K.tr(TR[:, k * 128:(k + 1) * 128], hb[:, k * 128:(k + 1) * 128], identb[:], [hb, identb], [TR])
                ht = hts[i % 2]
                K.cp(ht[:].rearrange("p k t -> p (k t)"), TR[:, :], [TR], [ht])
                K.dma(hT_d.ap().rearrange("(k p) t -> p k t", p=128)[:, :, i * 128:(i + 1) * 128], ht[:], [ht], [d_hT])
        S.barrier()
        if stop == 0:
            S.emit()
            return nc

        stopped = False
        with contextlib.ExitStack() as e1, contextlib.suppress(_Stop):
            wsb = sbt(e1, "wsb", [128, 8, NCOL], BF16)
            for k in range(8):
                K.dma(wsb[:, k, :], win[k * 128:(k + 1) * 128, :], [], [wsb], q="pool")
            cv = sbt(e1, "cv", [128, NCV], F32)
            K.dma(cv[:], cvec, [], [cv])
            omm = sbt(e1, "omm", [128, 9], F32)
            hmu = sbt(e1, "hmu", [128, 9], F32)
            omka = sbt(e1, "omka", [128, 2], F32)
            K.ts(omm[:], cv[:, 0:9], -1.0, ALU.mult, [cv], [omm], s2=1.0, op1=ALU.add)
            K.ts(hmu[:], cv[:, 0:9], 0.5, ALU.mult, [cv], [hmu])
            K.ts(omka[:], cv[:, 19:21], -1.0, ALU.mult, [cv], [omka], s2=1.0, op1=ALU.add)
            rv1 = sbt(e1, "rv1b", [128, 768], F32)
            K.dma(rv1[:], rowv1[:, 0:768], [], [rv1])
            lof = sbt(e1, "lof", [128, 896], F32)
            K.dma(lof[:], lora, [], [lof])
            lob = sbt(e1, "lob", [128, 896], BF16)
            K.cp(lob[:], lof[:], [lof], [lob], eng="dve")
            w2s = lambda ro, pr: lob[ro:ro + 64, pr * 128:(pr + 1) * 128]
            a2s = lambda ro, pr: lob[ro:ro + 64, 256 + pr * 128:256 + (pr + 1) * 128]
            g2s = lob[:, 512:768]
            gw2s = lambda ro: lob[ro:ro + 16, 768:896]

            yf = [sbt(e1, "yf%d" % i, [128, 512], BF16) for i in range(NT)]
            hw = [sbt(e1, "hw%d" % i, [128, 8, 516], BF16) for i in range(2)]
            tmp = [sbt(e1, "tmp%d" % i, [128, 512], F32) for i in range(9)]
            psb = sbt(e1, "psb", [128, 516], F32)
            rq = sbt(e1, "rq", [128, 512], F32)
            kq = sbt(e1, "kq", [128, 512], F32)
            vbf = sbt(e1, "vbf", [128, 512], BF16)
            twb = sbt(e1, "twb", [128, 512], BF16)
            alb = sbt(e1, "alb", [128, 512], BF16)
            sgb = sbt(e1, "sgb", [128, 512], BF16)
            glb = sbt(e1, "glb", [64, 512], BF16)
            sqb = sbt(e1, "sqb", [128, 512], BF16)
            At = [sbt(e1, "At%d" % p, [128, 512], BF16) for p in range(2)]
            Rt = [sbt(e1, "Rt%d" % p, [128, 512], BF16) for p in range(3)]
            Bt = [sbt(e1, "Bt%d" % p, [128, 512], BF16) for p in range(2)]
            Kt = [sbt(e1, "Kt%d" % p, [128, 512], BF16) for p in range(3)]
            prod = [sbt(e1, "prod%d" % p, [128, 512], BF16) for p in range(2)]
            TMs = [[sbt(e1, "TMs%d_%d" % (p, c), [128, 384], BF16) for c in range(4)] for p in range(2)]
            KgTM = [sbt(e1, "KgTM%d" % c, [128, 128], BF16) for c in range(4)]
            VgTM = [sbt(e1, "VgTM%d" % c, [128, 256], BF16) for c in range(4)]
            ogTM = [sbt(e1, "ogTM%d" % c, [128, 256], BF16) for c in range(4)]
            gtot = [sbt(e1, "gtot%d" % p, [128, NT], F32) for p in range(3)]
            Z32 = [sbt(e1, "Z32_%d" % p, [128, 128], F32) for p in range(2)]
            Sb = [sbt(e1, "Sb%d" % p, [128, 128], BF16) for p in range(2)]
            Z32G = sbt(e1, "Z32G", [128, 256], F32)
            SbG = sbt(e1, "SbG", [128, 256], BF16)
            Gm = [sbt(e1, "Gm%d" % h, [128, 512], BF16) for h in range(2)]
            NL = [sbt(e1, "NL%d" % i, [128, 512], BF16) for i in range(2)]
            Qs = [sbt(e1, "Qs%d" % i, [128, 256], BF16) for i in range(2)]
            Wsb = sbt(e1, "Wsb", [128, 128], BF16)
            Usb = sbt(e1, "Usb", [128, 128], BF16)
            Agm = sbt(e1, "Agm", [128, 256], BF16)
            ycomb = sbt(e1, "ycomb", [128, 128], F32)
            ysq = sbt(e1, "ysq", [128, 256], F32)
            st1 = sbt(e1, "st1", [128, 2], F32)
            st2 = sbt(e1, "st2", [128, 2], F32)
            mean = sbt(e1, "mean", [128, 2], F32)
            var = sbt(e1, "var", [128, 2], F32)
            rstd = sbt(e1, "rstd1", [128, 2], F32)
            bon = sbt(e1, "bon", [128, 2], F32)
            mixTM = sbt(e1, "mixTM", [128, 512], BF16)
            mixF = sbt(e1, "mixF", [128, 512], F32)
            mixT = [sbt(e1, "mixT%d" % i, [128, 4, 128], BF16) for i in range(2)]
            gnb = sbt(e1, "gnb", [128, 1], F32)
            rmb = sbt(e1, "rmb", [128, 1], F32)
            K.memset(gnb[:], 64e-5, [gnb])
            K.memset(rmb[:], 1e-6, [rmb])

            pj = [PJ0, PJ1]
            pji = [0]

            def next_pj():
                pji[0] ^= 1
                return pj[pji[0]]

            hT_v = hT_d.ap().rearrange("(k p) t -> p k t", p=128)

            def load_window(blk, buf):
                c0 = blk * 512
                lo = max(c0 - 2, 0)
                hi = min(c0 + 514, T)
                if c0 == 0:
                    K.memset(buf[:, :, 0:2], 0.0, [buf])
                if c0 + 512 == T:
                    K.memset(buf[:, :, 514:516], 0.0, [buf])
                for k in range(8):
                    K.dma(buf[:, k, lo - (c0 - 2):hi - (c0 - 2)], hT_v[:, k, lo:hi], [d_hT], [buf])

            def project_fm(buf, g, ncols, shift):
                ps = next_pj()
                cs = g * 128
                for k in range(8):
                    K.mm(ps[0:ncols, :], wsb[:, k, cs:cs + ncols], buf[:, k, 2:514], k == 0, k == 7, [wsb, buf], [ps])
                if shift:
                    for side in range(2):
                        col = 0 if side == 0 else 514
                        for k in range(8):
                            K.mm(WUY[0:ncols, 448 + 4 * g + 2 * side:448 + 4 * g + 2 * side + 2], wsb[:, k, cs:cs + ncols],
                                 buf[:, k, col:col + 2], k == 0, k == 7, [wsb, buf], [WUY])
                return ps

            def shifted(buf, g, outt, out_ap):
                ps = project_fm(buf, g, 128, True)
                K.cp(psb[:, 2:514], ps[:, :], [ps], [psb])
                K.cp(psb[:, 0:2], WUY[:, 448 + 4 * g:448 + 4 * g + 2], [WUY], [psb])
                K.cp(psb[:, 514:516], WUY[:, 448 + 4 * g + 2:448 + 4 * g + 4], [WUY], [psb])
                t1, pm = tmp[7], tmp[8]
                K.tt(t1[:], psb[:, 1:513], psb[:, 3:515], ALU.add, [psb], [t1], eng="pool")
                K.ts(pm[:], psb[:, 2:514], omm[:, g:g + 1], ALU.mult, [psb, omm], [pm])
                K.stt(out_ap, t1[:], hmu[:, g:g + 1], pm[:], ALU.mult, ALU.add, [t1, pm, hmu], [outt])

            def decay_exps(sdat, cs, fwd, c0chunk, gt, EA, EB, EC):
                P, Pex = tmp[5], tmp[6]
                K.scan(P[:], rmask[:], sdat[:], [rmask, sdat], [P])
                K.tt(Pex[:], P[:], sdat[:], ALU.subtract, [P, sdat], [Pex], eng="pool")
                K.act(gt[:, c0chunk:c0chunk + 4], P[:, 127:512:128], AF.Exp, [P], [gt], scale=cs)
                if fwd:
                    K.act(EA[:], P[:], AF.Exp, [P], [EA], scale=cs)
                    K.act(EB[:], P[:], AF.Exp, [P], [EB], scale=-cs)
                    K.act(EC[:], Pex[:], AF.Exp, [Pex], [EC], scale=cs)
                else:
                    K.act(EA[:], Pex[:], AF.Exp, [Pex], [EA], scale=-cs)
                    K.act(EB[:], Pex[:], AF.Exp, [Pex], [EB], scale=cs)
                    K.act(EC[:], P[:], AF.Exp, [P], [EC], scale=-cs)

            for pas in range(2):
                fwd = (pas == 0)
                ro = 0 if fwd else 64
                if pas == 1 and stop == 4:
                    raise _Stop()
                if pas == 1:
                    S.barrier()
                for p in range(2):
                    K.memset(Z32[p][:], 0.0, [Z32[p]])
                K.memset(Z32G[:], 0.0, [Z32G])
                blocks = list(range(NBLK)) if fwd else list(range(NBLK - 1, -1, -1))
                for bi, blk in enumerate(blocks):
                    buf = hw[bi % 2]
                    if stop == 36 and bi == 1:
                        raise _Stop()
                    load_window(blk, buf)
                    cch = blk * 4
                    shifted(buf, 6, tmp[0], tmp[0][:])
                    K.act(twb[:], tmp[0][:], AF.Tanh, [tmp[0]], [twb])
                    shifted(buf, 7, alb, alb[:])
                    if not fwd:
                        shifted(buf, 8, tmp[0], tmp[0][:])
                        K.act(sgb[:], tmp[0][:], AF.Sigmoid, [tmp[0]], [sgb])
                    for pr in range(2):
                        shifted(buf, 0 + pr, rq, rq[:])
                        shifted(buf, 2 + pr, kq, kq[:])
                        shifted(buf, 4 + pr, vbf, vbf[:])
                        sdat, EA, EB, EC, av = tmp[0], tmp[1], tmp[2], tmp[3], tmp[4]
                        ps = next_pj()
                        K.mm(ps[:, :], w2s(ro, pr), twb[ro:ro + 64, :], True, True, [lob, twb], [ps])
                        K.act(sdat[:], ps[:, :], AF.Sigmoid, [ps, cv], [sdat], bias=cv[:, (9 if fwd else 11) + pr:(9 if fwd else 11) + pr + 1])
                        decay_exps(sdat, CS_RW, fwd, cch, gtot[pr], EA, EB, EC)
                        ps = next_pj()
                        K.mm(ps[:, :], a2s(ro, pr), alb[ro:ro + 64, :], True, True, [lob, alb], [ps])
                        K.act(av[:], ps[:, :], AF.Sigmoid, [ps, cv], [av], bias=cv[:, (13 if fwd else 15) + pr:(13 if fwd else 15) + pr + 1])
                        kkr, kk = tmp[5], tmp[6]
                        K.ts(kkr[:], kq[:], cv[:, 17 + pr:18 + pr], ALU.mult, [kq, cv], [kkr])
                        K.tt(sqb[:], kkr[:], kkr[:], ALU.mult, [kkr], [sqb], eng="pool")
                        ps = next_pj()
                        K.mm(ps[:, :], bones[:], sqb[:], True, True, [bones, sqb], [ps])
                        K.act(tmp[7][:], ps[:, :], AF.Ln, [ps], [tmp[7]])
                        K.act(tmp[7][:], tmp[7][:], AF.Exp, [tmp[7]], [tmp[7]], scale=-0.5)
                        K.tt(kk[:], kkr[:], tmp[7][:], ALU.mult, [kkr, tmp[7]], [kk])
                        K.tt(Rt[pr][:], rq[:], EA[:], ALU.mult, [rq, EA], [Rt[pr]])
                        K.stt(At[pr][:], kk[:], -1.0, EC[:], ALU.mult, ALU.mult, [kk, EC], [At[pr]])
                        K.tt(tmp[7][:], kk[:], av[:], ALU.mult, [kk, av], [tmp[7]], eng="pool")
                        K.tt(Bt[pr][:], tmp[7][:], EB[:], ALU.mult, [tmp[7], EB], [Bt[pr]])
                        f1 = tmp[8]
                        K.ts(f1[:], av[:], cv[:, 19 + pr:20 + pr], ALU.mult, [av, cv, omka], [f1], s2=omka[:, pr:pr + 1], op1=ALU.add)
                        K.tt(tmp[7][:], kq[:], f1[:], ALU.mult, [kq, f1], [tmp[7]], eng="pool")
                        K.tt(Kt[pr][:], tmp[7][:], EB[:], ALU.mult, [tmp[7], EB], [Kt[pr]])
                        if not fwd:
                            ps = next_pj()
                            K.mm(ps[:, :], a2s(0, pr), alb[0:64, :], True, True, [lob, alb], [ps])
                            K.act(tmp[7][:], ps[:, :], AF.Sigmoid, [ps, cv], [tmp[7]], bias=cv[:, 13 + pr:14 + pr])
                            K.ts(tmp[7][:], tmp[7][:], cv[:, 19 + pr:20 + pr], ALU.mult, [tmp[7], cv, omka], [tmp[7]], s2=omka[:, pr:pr + 1], op1=ALU.add)
                            K.tt(f1[:], f1[:], tmp[7][:], ALU.add, [f1, tmp[7]], [f1])
                            K.tt(f1[:], f1[:], kq[:], ALU.mult, [f1, kq], [f1])
                            K.stt(prod[pr][:], rq[:], cv[:, 21 + pr:22 + pr], f1[:], ALU.mult, ALU.mult, [rq, cv, f1], [prod[pr]])
                        for c in range(4):
                            sl = slice(c * 128, (c + 1) * 128)
                            K.tr(TR[:, 0:128], vbf[:, sl], identb[:], [vbf, identb], [TR])
                            K.tr(TR[:, 128:256], Bt[pr][:, sl], identb[:], [Bt[pr], identb], [TR])
                            K.tr(TR[:, 256:384], Kt[pr][:, sl], identb[:], [Kt[pr], identb], [TR])
                            K.cp(TMs[pr][c][:], TR[:, 0:384], [TR], [TMs[pr][c]])
                    ps = project_fm(buf, 9, 128, False)
                    K.cp(rq[:], ps[:, :], [ps], [rq])
                    ps = project_fm(buf, 10, 128, False)
                    K.cp(kq[:], ps[:, :], [ps], [kq])
                    ps = next_pj()
                    for k in range(8):
                        K.mm(ps[0:64, :], wsb[:, k, 1408:1472], buf[:, k, 2:514], k == 0, k == 7, [wsb, buf], [ps])
                    K.cp(glb[:], ps[0:64, :], [ps], [glb])
                    rog = 0 if fwd else 32
                    ps = next_pj()
                    K.mm(ps[:, :], gw2s(rog), glb[rog:rog + 16, :], True, True, [lob, glb], [ps])
                    sdat, EA, EB, EC = tmp[0], tmp[1], tmp[2], tmp[3]
                    K.act(sdat[:], ps[:, :], AF.Sigmoid, [ps, cv], [sdat], bias=cv[:, (23 if fwd else 24):(24 if fwd else 25)])
                    K.act(sdat[:], sdat[:], AF.Ln, [sdat], [sdat])
                    decay_exps(sdat, CS_GL, fwd, cch, gtot[2], EA, EB, EC)
                    K.stt(Rt[2][:], rq[:], 0.125, EA[:], ALU.mult, ALU.mult, [rq, EA], [Rt[2]])
                    K.tt(Kt[2][:], kq[:], EB[:], ALU.mult, [kq, EB], [Kt[2]])
                    for c in range(4):
                        sl = slice(c * 128, (c + 1) * 128)
                        K.tr(TR[:, 0:128], Kt[2][:, sl], identb[:], [Kt[2], identb], [TR])
                        K.cp(KgTM[c][:], TR[:, 0:128], [TR], [KgTM[c]])
                        ps = next_pj()
                        for k in range(8):
                            K.mm(ps[:, 0:256], buf[:, k, 2 + c * 128:2 + (c + 1) * 128], wsb[:, k, 1472:1728], k == 0, k == 7, [wsb, buf], [ps])
                        if not fwd:
                            for k in range(8):
                                K.mm(ps[:, 256:512], buf[:, k, 2 + c * 128:2 + (c + 1) * 128], wsb[:, k, 1728:1984], k == 0, k == 7, [wsb, buf], [ps])
                        K.cp(VgTM[c][:], ps[:, 0:256], [ps], [VgTM[c]])
                        if not fwd:
                            K.act(ogTM[c][:], ps[:, 256:512], AF.Silu, [ps], [ogTM[c]])

                    if stop == 1:
                        raise _Stop()
                    if stop == 5 and not fwd:
                        raise _Stop()
                    corder = list(range(4)) if fwd else [3, 2, 1, 0]
                    mk = maskF if fwd else maskB
                    mkL = maskLF if fwd else maskLB
                    for ci, c in enumerate(corder):
                        if stop == 37 and ci == 1:
                            raise _Stop()
                        gch = cch + c
                        sl = slice(c * 128, (c + 1) * 128)
                        if fwd:
                            gcol = max(gch - 1, 0)
                        else:
                            gcol = gch
                        for pr in range(2):
                            Gp = [G0, G1]
                            tm = TMs[pr][c]
                            for h in range(2):
                                hs = slice(64 * h, 64 * h + 64)
                                K.mm(Gp[h][:, 0:128], Bt[pr][hs, sl], At[pr][hs, sl], True, True, [Bt[pr], At[pr]], [Gp[h]])
                                K.mm(Gp[h][:, 128:256], Kt[pr][hs, sl], At[pr][hs, sl], True, True, [Kt[pr], At[pr]], [Gp[h]])
                                K.mm(Gp[h][:, 256:384], Bt[pr][hs, sl], Rt[pr][hs, sl], True, True, [Bt[pr], Rt[pr]], [Gp[h]])
                                K.mm(Gp[h][:, 384:512], Kt[pr][hs, sl], Rt[pr][hs, sl], True, True, [Kt[pr], Rt[pr]], [Gp[h]])
                                K.mm([IA, IB][h][:, 256:384], At[pr][hs, sl], Bt[pr][hs, sl], True, True, [Bt[pr], At[pr]], [[IA, IB][h]])
                            for h in range(2):
                                K.tt(Gm[h][:], Gp[h][:, :], mk[:], ALU.mult, [Gp[h], mk], [Gm[h]])
                            nl, q = NL[0], Qs[0]
                            for h in range(2):
                                K.cp(nl[:, 256 * h:256 * h + 128], Gm[h][:, 0:128], [Gm[h]], [nl])
                                K.tt(q[:, 128 * h:128 * h + 128], Gm[h][:, 0:128], identb[:], ALU.add, [Gm[h], identb], [q], eng="pool")
                            for h in range(2):
                                K.tt(nl[:, 256 * h + 128:256 * h + 256], [IA, IB][h][:, 256:384], mkL[:, 0:128], ALU.mult, [[IA, IB][h], mkL], [nl])
                            cur = 0
                            for r in range(1, 8):
                                nl, q = NL[cur], Qs[cur]
                                nl2, q2 = NL[cur ^ 1], Qs[cur ^ 1]
                                for h in range(2):
                                    Nh = nl[:, 256 * h:256 * h + 128]
                                    Lh = nl[:, 256 * h + 128:256 * h + 256]
                                    if r < 7:
                                        K.mm(IA[:, 256 * h:256 * h + 128], Lh, Nh, True, True, [nl], [IA])
                                        K.mm(IA[:, 256 * h + 128:256 * h + 256], Nh, Lh, True, True, [nl], [IA])
                                    if r >= 2:
                                        K.mm(IB[:, 128 * h:128 * h + 128], Lh, q[:, 128 * h:128 * h + 128], True, True, [nl, q], [IB])
                                if r < 7:
                                    K.cp(nl2[:], IA[:, :], [IA], [nl2])
                                if r >= 2:
                                    K.tt(q2[:], IB[:, 0:256], q[:], ALU.add, [IB, q], [q2])
                                    qfin = q2
                                    if r < 7:
                                        cur ^= 1
                                else:
                                    K.cp(q2[:], q[:], [q], [q2])
                                    cur ^= 1
                            Minv = qfin
                            if stop == 2:
                                raise _Stop()
                            if stop == 38 and ci == 1:
                                raise _Stop()
                            K.ts(Sb[pr][:], Z32[pr][:], gtot[pr][:, gcol:gcol + 1], ALU.mult, [Z32[pr], gtot[pr]], [Sb[pr]])
                            K.mm(WUY[:, 0:128], At[pr][:, sl], Sb[pr][:], True, False, [At[pr], Sb[pr]], [WUY])
                            for h in range(2):
                                K.mm(WUY[:, 64 * h:64 * h + 64], Gm[h][:, 128:256], tm[:, 64 * h:64 * h + 64], False, h == 1, [Gm[h], tm], [WUY])
                            K.cp(Wsb[:], WUY[:, 0:128], [WUY], [Wsb])
                            for h in range(2):
                                K.mm(WUY[:, 128 + 64 * h:128 + 64 * h + 64], Minv[:, 128 * h:128 * h + 128], Wsb[:, 64 * h:64 * h + 64], True, True, [Minv, Wsb], [WUY])
                            K.cp(Usb[:], WUY[:, 128:256], [WUY], [Usb], eng="dve")
                            K.mm(WUY[:, 256:384], Rt[pr][:, sl], Sb[pr][:], True, False, [Rt[pr], Sb[pr]], [WUY])
                            for h in range(2):
                                yo = WUY[:, 256 + 64 * h:256 + 64 * h + 64]
                                K.mm(yo, Gm[h][:, 256:384], Usb[:, 64 * h:64 * h + 64], False, False, [Gm[h], Usb], [WUY])
                                K.mm(yo, Gm[h][:, 384:512], tm[:, 64 * h:64 * h + 64], False, h == 1, [Gm[h], tm], [WUY])
                            K.mm(WUY[:, 384:512], tm[:, 128:256], Usb[:], True, False, [tm, Usb], [WUY])
                            K.mm(WUY[:, 384:512], tm[:, 256:384], tm[:, 0:128], False, True, [tm], [WUY])
                            for h in range(2):
                                hs = slice(64 * h, 64 * h + 64)
                                K.stt(Z32[pr][hs, hs], Z32[pr][hs, hs], gtot[pr][hs, gcol:gcol + 1], WUY[hs, 384 + 64 * h:384 + 64 * h + 64], ALU.mult, ALU.add, [Z32[pr], gtot[pr], WUY], [Z32[pr]])
                            if stop == 25:
                                raise _Stop()
                            if fwd:
                                K.cp(yf[gch][:, pr * 128:(pr + 1) * 128], WUY[:, 256:384], [WUY], [yf[gch]])
                            else:
                                K.tt(ycomb[:], WUY[:, 256:384], yf[gch][:, pr * 128:(pr + 1) * 128], ALU.add, [WUY, yf[gch]], [ycomb])
                                y3 = ycomb[:].rearrange("p (h x) -> p h x", h=2)
                                K.red(st1[:], y3, ALU.add, [ycomb], [st1])
                                K.tt(ysq[:, 0:128], ycomb[:], ycomb[:], ALU.mult, [ycomb], [ysq])
                                K.red(st2[:], ysq[:, 0:128].rearrange("p (h x) -> p h x", h=2), ALU.add, [ysq], [st2])
                                K.ts(mean[:], st1[:], 1.0 / 64, ALU.mult, [st1], [mean])
                                K.tt(var[:], mean[:], mean[:], ALU.mult, [mean], [var])
                                K.stt(var[:], st2[:], 1.0 / 64, var[:], ALU.mult, ALU.subtract, [st2, var], [var])
                                K.act(var[:], var[:], AF.Sqrt, [var, gnb], [var], bias=gnb[:])
                                K.recip(rstd[:], var[:], [var], [rstd])
                                K.mm(IB[:, 0:2], prod[pr][:, sl], hsel[:], True, True, [prod[pr], hsel], [IB])
                                K.cp(bon[:], IB[:, 0:2], [IB], [bon], eng="dve")
                                for h in range(2):
                                    hs = slice(64 * h, 64 * h + 64)
                                    K.ts(ycomb[:, hs], ycomb[:, hs], mean[:, h:h + 1], ALU.subtract, [ycomb, mean, rstd], [ycomb], s2=rstd[:, h:h + 1], op1=ALU.mult)
                                lw = rv1[:, pr * 128:(pr + 1) * 128]
                                lb = rv1[:, 256 + pr * 128:256 + (pr + 1) * 128]
                                K.tt(ycomb[:], ycomb[:], lw, ALU.mult, [ycomb, rv1], [ycomb])
                                K.tt(ycomb[:], ycomb[:], lb, ALU.add, [ycomb, rv1], [ycomb])
                                for h in range(2):
                                    hs = slice(64 * h, 64 * h + 64)
                                    K.stt(ycomb[:, hs], tm[:, hs], bon[:, h:h + 1], ycomb[:, hs], ALU.mult, ALU.add, [tm, bon, ycomb], [ycomb])
                                if pr == 0:
                                    K.mm(G0[:, 0:256], sgb[:, sl], g2s, True, True, [sgb, lob], [G0])
                                    K.cp(ysq[:], G0[:, 0:256], [G0], [ysq])
                                K.tt(mixTM[:, pr * 128:(pr + 1) * 128], ycomb[:], ysq[:, pr * 128:(pr + 1) * 128], ALU.mult, [ycomb, ysq], [mixTM])
                        if stop == 3:
                            raise _Stop()
                        for h in range(2):
                            hs = slice(64 * h, 64 * h + 64)
                            K.mm([G0, G1][h][:, 0:128], Kt[2][hs, sl], Rt[2][hs, sl], True, True, [Kt[2], Rt[2]], [[G0, G1][h]])
                        for h in range(2):
                            K.tt(Agm[:, 128 * h:128 * h + 128], [G0, G1][h][:, 0:128], mk[:, 256:384], ALU.mult, [[G0, G1][h], mk], [Agm])
                        K.ts(SbG[:], Z32G[:], gtot[2][:, gcol:gcol + 1], ALU.mult, [Z32G, gtot[2]], [SbG])
                        K.mm(WUY[:, 0:256], Rt[2][:, sl], SbG[:], True, False, [Rt[2], SbG], [WUY])
                        for h in range(2):
                            K.mm(WUY[:, 128 * h:128 * h + 128], Agm[:, 128 * h:128 * h + 128], VgTM[c][:, 128 * h:128 * h + 128], False, h == 1, [Agm, VgTM[c]], [WUY])
                        K.mm(WUY[:, 256:512], KgTM[c][:], VgTM[c][:], True, True, [KgTM[c], VgTM[c]], [WUY])
                        for h in range(2):
                            hs = slice(64 * h, 64 * h + 64)
                            vs = slice(128 * h, 128 * h + 128)
                            K.stt(Z32G[hs, vs], Z32G[hs, vs], gtot[2][hs, gcol:gcol + 1], WUY[hs, 256 + 128 * h:256 + 128 * h + 128], ALU.mult, ALU.add, [Z32G, gtot[2], WUY], [Z32G])
                        if stop == 35:
                            raise _Stop()
                        if fwd:
                            K.cp(yf[gch][:, 256:512], WUY[:, 0:256], [WUY], [yf[gch]], eng="dve")
                        else:
                            K.tt(ysq[:], WUY[:, 0:256], yf[gch][:, 256:512], ALU.add, [WUY, yf[gch]], [ysq])
                            K.tt(mixF[:, 0:256], ysq[:], ysq[:], ALU.mult, [ysq], [mixF])
                            K.red(st2[:], mixF[:, 0:256].rearrange("p (h x) -> p h x", h=2), ALU.add, [mixF], [st2])
                            K.act(var[:], st2[:], AF.Sqrt, [st2, rmb], [var], scale=1.0 / 128, bias=rmb[:])
                            K.recip(rstd[:], var[:], [var], [rstd])
                            for h in range(2):
                                hs = slice(128 * h, 128 * h + 128)
                                K.stt(ysq[:, hs], ysq[:, hs], rstd[:, h:h + 1], rv1[:, 512 + 128 * h:512 + 128 * h + 128], ALU.mult, ALU.mult, [ysq, rstd, rv1], [ysq])
                            K.tt(mixTM[:, 256:512], ysq[:], ogTM[c][:], ALU.mult, [ysq, ogTM[c]], [mixTM])
                            mt = mixT[gch % 2]
                            for kc in range(4):
                                K.tr(TR[:, 512 + kc * 128:512 + (kc + 1) * 128], mixTM[:, kc * 128:(kc + 1) * 128], identb[:], [mixTM, identb], [TR])
                            K.cp(mt[:].rearrange("p k t -> p (k t)"), TR[:, 512:1024], [TR], [mt])
                            tq_, to_ = (gch * 128) // QS, (gch * 128) % QS
                            K.dma(cc_in[tq_].ap().rearrange("(k p) t -> p k t", p=128)[:, :, to_:to_ + 128], mt[:], [mt], [d_ccin])
                            if debug:
                                K.cp(mixF[:], mixTM[:], [mixTM], [mixF], eng="dve")
                                K.dma(dbg_mix[gch * 128:(gch + 1) * 128, :], mixF[:], [mixF], [])
        S.barrier()
        if debug == 1 or stop < 99:
            S.emit()
            return nc

        for q_ in range(NQ):
            S.op("pool", (lambda q_: lambda e: e.collective_compute("AllGather", ALU.bypass, replica_groups=[[0, 1], [2, 3], [4, 5], [6, 7]],
                                                                    ins=[cc_in[q_].ap()], outs=[cc_out[q_].ap()]))(q_), [d_ccin], [d_ccout])
        S.barrier()

        with contextlib.ExitStack() as e2:
            rv2 = sbt(e2, "rv2", [128, 2 * D + 36 + 2], F32)
            K.dma(rv2[:], rowv2, [], [rv2])
            acc = [sbt(e2, "acc%d" % i, [128, D], F32) for i in range(NT2)]
            h2T = sbt(e2, "h2T", [128, 8, TH], BF16)
            comb = [sbt(e2, "comb%d" % i, [128, 32], F32) for i in range(NT2)]
            ss = sbt(e2, "ss2", [128, 1], F32)
            sd = sbt(e2, "sd2", [128, 1], F32)
            rstd = sbt(e2, "rstd2", [128, 1], F32)
            epsb = sbt(e2, "epsb2", [128, 1], F32)
            K.memset(epsb[:], 1e-6, [epsb])
            junk = sbt(e2, "junk2", [128, D], BF16)
            with contextlib.ExitStack() as e2a:
                wof = sbt(e2a, "wof", [128, 8, D], BF16)
                for k in range(8):
                    K.dma(wof[:, k, :], wo[k * 128:(k + 1) * 128, :], [], [wof], q="pool")
                wrs = sbt(e2a, "wrs", [128, 8, 36], F32)
                K.dma(wrs[:], wr.rearrange("(k p) e -> p k e", p=128), [], [wrs])
                mA = [sbt(e2a, "mA%d" % i, [128, 8, 128], BF16) for i in range(2)]
                mB = [sbt(e2a, "mB%d" % i, [128, 8, 128], BF16) for i in range(2)]
                msel = [sbt(e2a, "msel%d" % i, [128, 8, 128], BF16) for i in range(2)]
                xr = [sbt(e2a, "xr%d" % i, [128, D], F32) for i in range(2)]
                hn = sbt(e2a, "hn", [128, D], F32)
                hTf = sbt(e2a, "hTf", [128, 8, 128], F32)
                lg = sbt(e2a, "lg", [128, 36], F32)
                m8 = sbt(e2a, "m8", [128, 8], F32)
                cmx = sbt(e2a, "cmx", [128, 1], F32)
                oh = sbt(e2a, "oh", [128, 4], F32)
                ex4 = sbt(e2a, "ex4", [128, 4], F32)
                pg = sbt(e2a, "pg", [128, 1], F32)
                fs = sbt(e2a, "fs", [128, 8], F32)
                e2v = sbt(e2a, "e2v", [128, 1], F32)
                c1 = sbt(e2a, "c1", [128, 1], F32)
                c2 = sbt(e2a, "c2", [128, 1], F32)
                k1 = sbt(e2a, "k1", [128, 8], F32)
                k2 = sbt(e2a, "k2", [128, 8], F32)
                c8 = sbt(e2a, "c8", [128, 8], F32)
                ccv = [cc_out[q_].ap().rearrange("(k p) t -> p k t", p=128) for q_ in range(NQ)]
                sel0 = rv2[:, 2 * D + 36:2 * D + 37]
                sel1 = rv2[:, 2 * D + 37:2 * D + 38]
                for i in range(NT2):
                    a_, b_, m_ = mA[i % 2], mB[i % 2], msel[i % 2]
                    tA, tB = i * 128, TH + i * 128
                    K.dma(a_[:], ccv[tA // QS][:, :, tA % QS:tA % QS + 128], [d_ccout], [a_])
                    K.dma(b_[:], ccv[tB // QS][:, :, tB % QS:tB % QS + 128], [d_ccout], [b_])
                    x_ = xr[i % 2]
                    K.dma(x_[:], xhalf[i * 128:(i + 1) * 128, :], [], [x_])
                    K.ts(m_[:], a_[:], sel0, ALU.mult, [a_, rv2], [m_])
                    K.stt(m_[:], b_[:], sel1, m_[:], ALU.mult, ALU.add, [b_, rv2, m_], [m_])
                    for half in range(2):
                        ps = pj[half]
                        for k in range(8):
                            K.mm(ps[:, :], m_[:, k, :], wof[:, k, half * 512:(half + 1) * 512], k == 0, k == 7, [m_, wof], [ps])
                        K.tt(acc[i][:, half * 512:(half + 1) * 512], ps[:, :], x_[:, half * 512:(half + 1) * 512], ALU.add, [ps, x_], [acc[i]])
                    K.act(junk[:], acc[i][:], AF.Square, [acc[i]], [junk, ss], accum=ss[:])
                    K.act(sd[:], ss[:], AF.Sqrt, [ss, epsb], [sd], scale=1.0 / D, bias=epsb[:])
                    K.recip(rstd[:], sd[:], [sd], [rstd])
                    K.stt(hn[:], acc[i][:], rstd[:, 0:1], rv2[:, 0:D], ALU.mult, ALU.mult, [acc[i], rstd, rv2], [hn])
                    for half in range(2):
                        ps = [G0, G1][half]
                        for k4 in range(4):
                            k = half * 4 + k4
                            K.tr(ps[:, k4 * 128:(k4 + 1) * 128], hn[:, k * 128:(k + 1) * 128], identf[:], [hn, identf], [ps])
                        K.cp(hTf[:, half * 4:half * 4 + 4, :], ps[:, :].rearrange("p (k t) -> p k t", k=4), [ps], [hTf])
                        K.cp(h2T[:, half * 4:half * 4 + 4, i * 128:(i + 1) * 128], ps[:, :].rearrange("p (k t) -> p k t", k=4), [ps], [h2T], eng="dve")
                    for k in range(8):
                        K.mm(IA[:, 0:36], hTf[:, k, :], wrs[:, k, :], k == 0, k == 7, [hTf, wrs], [IA])
                    K.tt(lg[:], IA[:, 0:36], rv2[:, 2 * D:2 * D + 36], ALU.add, [IA, rv2], [lg])
                    K.red(cmx[:], lg[:, 0:4], ALU.max, [lg], [cmx])
                    K.ts(oh[:], lg[:, 0:4], cmx[:, 0:1], ALU.is_equal, [lg, cmx], [oh])
                    K.ts(ex4[:], lg[:, 0:4], cmx[:, 0:1], ALU.subtract, [lg, cmx], [ex4])
                    K.act(ex4[:], ex4[:], AF.Exp, [ex4], [ex4])
                    K.red(pg[:], ex4[:], ALU.add, [ex4], [pg])
                    K.recip(pg[:], pg[:], [pg], [pg])
                    K.ts(fs[:], lg[:, 4:12], oh[:, 0:1], ALU.mult, [lg, oh], [fs])
                    for g in range(1, 4):
                        K.stt(fs[:], lg[:, 4 + 8 * g:12 + 8 * g], oh[:, g:g + 1], fs[:], ALU.mult, ALU.add, [lg, oh, fs], [fs])
                    S.op("dve", lambda e: e.max(out=m8[:], in_=fs[:]), [fs.d], [m8.d])
                    K.ts(k1[:], fs[:], m8[:, 0:1], ALU.is_equal, [fs, m8], [k1])
                    K.ts(k2[:], fs[:], m8[:, 1:2], ALU.is_equal, [fs, m8], [k2])
                    K.tt(e2v[:], m8[:, 1:2], m8[:, 0:1], ALU.subtract, [m8], [e2v])
                    K.act(e2v[:], e2v[:], AF.Exp, [e2v], [e2v])
                    K.ts(c1[:], e2v[:], 1.0, ALU.add, [e2v], [c1])
                    K.recip(c1[:], c1[:], [c1], [c1])
                    K.tt(c1[:], c1[:], pg[:], ALU.mult, [c1, pg], [c1])
                    K.tt(c2[:], c1[:], e2v[:], ALU.mult, [c1, e2v], [c2])
                    K.ts(c8[:], k1[:], c1[:, 0:1], ALU.mult, [k1, c1], [c8])
                    K.stt(c8[:], k2[:], c2[:, 0:1], c8[:], ALU.mult, ALU.add, [k2, c2, c8], [c8])
                    for g in range(4):
                        K.ts(comb[i][:, 8 * g:8 * g + 8], c8[:], oh[:, g:g + 1], ALU.mult, [c8, oh], [comb[i]])
            S.barrier()
            with contextlib.ExitStack() as e2b:
                wgs = [sbt(e2b, "wgs%d" % i, [128, 8, DEXP], BF16) for i in range(2)]
                wus = [sbt(e2b, "wus%d" % i, [128, 8, DEXP], BF16) for i in range(2)]
                wds = [sbt(e2b, "wds%d" % i, [128, 4, D], BF16) for i in range(2)]
                actT = [sbt(e2b, "actT%d" % i, [128, 4, 512], BF16) for i in range(2)]
                sil = [sbt(e2b, "sil%d" % i, [128, 512], BF16) for i in range(2)]
                NB2 = TH // 512
                pbank = [PJ0, PJ1, G0, G1, IA, IB, WUY]
                pbi = [0]

                def nb():
                    pbi[0] = (pbi[0] + 1) % len(pbank)
                    return pbank[pbi[0]]
                for ex in range(NEXP):
                    if ex % 8 == 0 and ex > 0:
                        S.barrier()
                    wg_, wu_, wd_ = wgs[ex % 2], wus[ex % 2], wds[ex % 2]
                    for k in range(8):
                        K.dma(wg_[:, k, :], wg[ex, k * 128:(k + 1) * 128, :], [], [wg_], q="pool")
                        K.dma(wu_[:, k, :], wu[ex, k * 128:(k + 1) * 128, :], [], [wu_], q="pool")
                    for k in range(4):
                        K.dma(wd_[:, k, 0:512], wd[ex, k * 128:(k + 1) * 128, 0:512], [], [wd_], q="pool")
                        K.dma(wd_[:, k, 512:1024], wd[ex, k * 128:(k + 1) * 128, 512:1024], [], [wd_], q="pool")
                    for tb in range(NB2):
                        at = actT[tb % 2]
                        tsl = slice(tb * 512, (tb + 1) * 512)
                        for f in range(4):
                            pg_ = nb()
                            for k in range(8):
                                K.mm(pg_[:, :], wg_[:, k, f * 128:(f + 1) * 128], h2T[:, k, tsl], k == 0, k == 7, [wg_, h2T], [pg_])
                            pu_ = nb()
                            for k in range(8):
                                K.mm(pu_[:, :], wu_[:, k, f * 128:(f + 1) * 128], h2T[:, k, tsl], k == 0, k == 7, [wu_, h2T], [pu_])
                            sl_ = sil[f % 2]
                            K.act(sl_[:], pg_[:, :], AF.Silu, [pg_], [sl_])
                            K.tt(at[:, f, :], sl_[:], pu_[:, :], ALU.mult, [sl_, pu_], [at])
                        for tt_ in range(4):
                            ti = tb * 4 + tt_
                            for half in range(2):
                                py = nb()
                                for f in range(4):
                                    K.mm(py[:, :], at[:, f, tt_ * 128:(tt_ + 1) * 128], wd_[:, f, half * 512:(half + 1) * 512], f == 0, f == 3, [at, wd_], [py])
                                hsl = slice(half * 512, (half + 1) * 512)
                                K.stt(acc[ti][:, hsl], py[:, :], comb[ti][:, ex:ex + 1], acc[ti][:, hsl], ALU.mult, ALU.add, [py, comb[ti], acc[ti]], [acc[ti]])
                ob = [sbt(e2b, "ob%d" % i, [128, D], F32) for i in range(2)]
                for i in range(NT2):
                    K.act(junk[:], acc[i][:], AF.Square, [acc[i]], [junk, ss], accum=ss[:])
                    K.act(sd[:], ss[:], AF.Sqrt, [ss, epsb], [sd], scale=1.0 / D, bias=epsb[:])
                    K.recip(rstd[:], sd[:], [sd], [rstd])
                    o_ = ob[i % 2]
                    K.stt(o_[:], acc[i][:], rstd[:, 0:1], rv2[:, D:2 * D], ALU.mult, ALU.mult, [acc[i], rstd, rv2], [o_])
                    K.dma(out[i * 128:(i + 1) * 128, :], o_[:], [o_], [])
                S.barrier()
        S.emit()
    return nc


def _consts():
    j = np.arange(128)[:, None]
    t = np.arange(128)[None, :]
    su = (t > j).astype(np.float32)
    iu = (t >= j).astype(np.float32)
    sl = (t < j).astype(np.float32)
    il = (t <= j).astype(np.float32)
    maskF = np.concatenate([su, su, iu, iu], 1)
    maskB = np.concatenate([sl, sl, il, il], 1)
    maskLF = np.concatenate([sl, sl], 1)
    maskLB = np.concatenate([su, su], 1)
    ident = np.eye(128, dtype=np.float32)
    rmask = np.ones((128, 512), np.float32)
    rmask[:, 0::128] = 0.0
    bones = (j // 64 == t // 64).astype(np.float32)
    hsel = (j // 64 == np.arange(2)[None, :]).astype(np.float32)
    return np.concatenate([maskF, maskB, maskLF, maskLB, ident, rmask, bones, hsel], 1).astype(np.float32)


def _rep(v):
    return np.broadcast_to(np.asarray(v, np.float32)[None, :], (128, v.shape[0]))


def make_in_maps(inp, T):
    L = 0
    w_in = inp["w_in"][L]
    TH = T // 2
    cm = _consts()
    wo_full = inp["w_out"][L]
    wo_perm = np.concatenate([wo_full[0:256], wo_full[512:768], wo_full[256:512], wo_full[768:1024]], 0)
    wr = np.concatenate([inp["moe_w_coarse"][L], inp["moe_w_fine"][L]], 1)
    rbias = np.concatenate([inp["moe_b_coarse"][L], inp["moe_b_fine"][L]])
    maps = []
    for c in range(8):
        b, hh = c // 2, c % 2
        ch = slice(256 * hh, 256 * hh + 256)
        gk = slice(128 * hh, 128 * hh + 128)
        cols = []
        mus = []
        mu = inp["rw_mu"][L]

        def grp(idx):
            cols.append(w_in[:, idx])
            mus.append(mu[idx])
        c0 = 256 * hh
        for base in (0, 512, 1024):
            for pr in range(2):
                grp(np.arange(base + c0 + 128 * pr, base + c0 + 128 * pr + 128))
        grp(np.arange(1536, 1664))
        grp(np.arange(1664, 1792))
        grp(np.arange(1792, 1920))
        G = 1920
        cols.append(w_in[:, G + 128 * hh:G + 128 * hh + 128])
        cols.append(w_in[:, G + 256 + 128 * hh:G + 256 + 128 * hh + 128])
        z16 = np.zeros((D, 16), np.float32)
        cols.append(np.concatenate([w_in[:, G + 1024:G + 1040], z16, w_in[:, G + 1040:G + 1056], z16], 1))
        cols.append(w_in[:, G + 512 + 256 * hh:G + 512 + 256 * hh + 256])
        cols.append(w_in[:, G + 1056 + 256 * hh:G + 1056 + 256 * hh + 256])
        win = np.ascontiguousarray(np.concatenate(cols, 1))
        assert win.shape == (D, NCOL)
        cvl = list(mus)
        for nm in ("rw_w0_f", "rw_w0_b", "rw_a0_f", "rw_a0_b", "rw_k_k", "rw_k_a"):
            v = inp[nm][L][ch]
            cvl += [v[0:128], v[128:256]]
        rk = inp["rw_r_k"][L].reshape(-1)[ch]
        cvl += [rk[0:128], rk[128:256]]
        cvl += [inp["gla_gb_f"][L][gk], inp["gla_gb_b"][L][gk]]
        cvec = np.ascontiguousarray(np.stack(cvl, 1).astype(np.float32))
        assert cvec.shape == (128, NCV)
        rowv1 = np.ascontiguousarray(np.concatenate([_rep(inp["rw_ln_w"][L][ch]), _rep(inp["rw_ln_b"][L][ch]),
                                                     _rep(inp["gla_norm_w"][L][ch]), _rep(inp["ln1_w"][L])], 1))
        sel = np.zeros(2, np.float32)
        sel[hh] = 1.0
        rowv2 = np.ascontiguousarray(np.concatenate([_rep(inp["ln2_w"][L]), _rep(inp["ln_f_w"]), _rep(rbias), _rep(sel)], 1))
        gw2 = np.zeros((128, 128), np.float32)
        gw2[0:16] = inp["gla_gw2_f"][L][:, gk]
        gw2[32:48] = inp["gla_gw2_b"][L][:, gk]
        lora = np.ascontiguousarray(np.concatenate([
            np.concatenate([inp["rw_w2_f"][L][:, ch], inp["rw_w2_b"][L][:, ch]], 0),
            np.concatenate([inp["rw_a2_f"][L][:, ch], inp["rw_a2_b"][L][:, ch]], 0),
            inp["rw_g2"][L][:, ch], gw2], 1).astype(np.float32))
        xb = inp["x"][b]
        maps.append({
            "xfull": np.ascontiguousarray(xb[:T]),
            "xhalf": np.ascontiguousarray(xb[hh * TH:(hh + 1) * TH]),
            "win": win, "cvec": cvec, "rowv1": rowv1, "rowv2": rowv2, "lora": lora, "cmask": cm,
            "wo": np.ascontiguousarray(wo_perm), "wr": np.ascontiguousarray(wr),
            "wg": inp["moe_w_gate"][L], "wu": inp["moe_w_up"][L], "wd": inp["moe_w_down"][L],
        })
    return maps


_NC_CACHE = {}


def kernel(**inputs):
    inp = {k: np.asarray(v) for k, v in inputs.items()}
    T = inp["x"].shape[1]
    if T not in _NC_CACHE:
        _NC_CACHE[T] = build_nc(T)
    nc = _NC_CACHE[T]
    maps = make_in_maps(inp, T)
    res = run_bass_kernel_spmd(nc, maps, core_ids=list(range(8)))
    TH = T // 2
    out = np.zeros((4, T, D), np.float32)
    for c in range(8):
        b, hh = c // 2, c % 2
        out[b, hh * TH:(hh + 1) * TH] = np.asarray(res.results[c]["out"])
    return out
```
